# Optimizing a Trainium2 kernel written in Bass

```python
import math
import jax
import jax.numpy as jnp
from jax import lax
import numpy as np

D_MODEL = 1024
BATCH = 4
SEQ = 4096
DEPTH = 2

GRID_W = 64
CTX_LEN = 256
EPS = 1e-6
ROPE_BASE = 10000.0

DA_HEADS = 8
DA_DIM = 64
DA_VDIM = 2 * DA_DIM
DA_QK = DA_HEADS * 2 * DA_DIM
DA_W = DA_HEADS * DA_VDIM
Q_BLOCK = 128

DN_HEADS = 8
DN_DK = 128
DN_DV = 128
DN_QK = DN_HEADS * DN_DK
DN_W = DN_HEADS * DN_DV
DN_QKV = 2 * DN_QK + DN_W
DN_CONV = 5
DN_CHUNK = 64

IN_SIZES = (DA_QK, DA_QK, DA_W, DN_QKV, DN_W, 2 * DN_HEADS, 2 * DN_HEADS, D_MODEL, D_MODEL)
N_IN = sum(IN_SIZES)

PK_HEADS = 8
N_KEYS = 128
N_EXPERTS = N_KEYS * N_KEYS
PK_TOPK = 16
PK_DQ = 256
PK_HALF = PK_DQ // 2
TOK_BLOCK = 128

kernel_name = 'hybrid_diffattn_gdn_peer_dit'


def _rmsnorm(x, g):
    xf = x.astype(jnp.float32)
    y = xf * lax.rsqrt(jnp.mean(xf * xf, axis=-1, keepdims=True) + EPS)
    return (y * g.astype(jnp.float32)).astype(x.dtype)


def _modulate(x, g, shift, scale):
    return _rmsnorm(x, g) * (1 + scale) + shift


def _l2norm(x):
    xf = x.astype(jnp.float32)
    return xf * lax.rsqrt(jnp.sum(xf * xf, axis=-1, keepdims=True) + EPS)


def _split_cols(p):
    outs, start = [], 0
    for n in IN_SIZES:
        outs.append(p[..., start:start + n])
        start += n
    return outs


def _axial_rope(n_rows, dim):
    row = jnp.repeat(jnp.arange(n_rows, dtype=jnp.float32), GRID_W)
    col = jnp.tile(jnp.arange(GRID_W, dtype=jnp.float32), n_rows)
    n_freq = dim // 4
    inv = ROPE_BASE ** (-jnp.arange(n_freq, dtype=jnp.float32) / n_freq)
    ang = jnp.concatenate([row[:, None] * inv, col[:, None] * inv], axis=-1)
    return jnp.cos(ang), jnp.sin(ang)


def _apply_rope(x, cos, sin):
    x1 = x[..., 0::2].astype(jnp.float32)
    x2 = x[..., 1::2].astype(jnp.float32)
    cs = cos[None, :, None, None, :]
    sn = sin[None, :, None, None, :]
    y = jnp.stack([x1 * cs - x2 * sn, x1 * sn + x2 * cs], axis=-1).reshape(x.shape)
    return y.astype(x.dtype)


def _diff_attention(ql, kl, vl, qc, kc, vc, lam, subln, cos, sin, lam_init, need_ctx):
    B, S = ql.shape[:2]
    Lc = qc.shape[1]
    rs = lambda a: a.reshape(a.shape[0], a.shape[1], DA_HEADS, 2, DA_DIM)
    ql = _apply_rope(rs(ql), cos, sin)
    kl = _apply_rope(rs(kl), cos, sin)
    qc, kc = rs(qc), rs(kc)
    vl = vl.reshape(B, S, DA_HEADS, DA_VDIM)
    vc = vc.reshape(B, Lc, DA_HEADS, DA_VDIM)
    lf = lam.astype(jnp.float32)
    lam_val = jnp.exp(jnp.sum(lf[0] * lf[1])) - jnp.exp(jnp.sum(lf[2] * lf[3])) + lam_init
    k_all = jnp.concatenate([kc, kl], axis=1)
    v_all = jnp.concatenate([vc, vl], axis=1)

    def attend(q_blk, k, v):
        s = jnp.einsum('bqhcd,bkhcd->bhcqk', q_blk, k).astype(jnp.float32) * (DA_DIM ** -0.5)
        p = jax.nn.softmax(s, axis=-1)
        a = (p[:, :, 0] - lam_val * p[:, :, 1]).astype(v.dtype)
        return jnp.einsum('bhqk,bkhe->bqhe', a, v)

    def post(o):
        o = _rmsnorm(o, subln) * (1 - lam_init)
        return o.reshape(o.shape[0], o.shape[1], DA_W)

    nblk = S // Q_BLOCK
    qb = jnp.moveaxis(ql.reshape(B, nblk, Q_BLOCK, DA_HEADS, 2, DA_DIM), 1, 0)
    ol = lax.map(lambda q_blk: attend(q_blk, k_all, v_all), qb)
    ol = jnp.moveaxis(ol, 0, 1).reshape(B, S, DA_HEADS, DA_VDIM)
    out_l = post(ol)
    out_c = post(attend(qc, kc, vc)) if need_ctx else None
    return out_l, out_c


def _short_conv(x, w):
    C = x.shape[-1]
    pad = DN_CONV // 2
    y = lax.conv_general_dilated(x, w[:, None, :].astype(x.dtype), window_strides=(1,),
                                 padding=((pad, pad),), dimension_numbers=('NWC', 'WIO', 'NWC'),
                                 feature_group_count=C)
    return jax.nn.silu(y)


def _dn_prep(qkv, b_raw, a_raw, conv_w, a_log, dt_bias):
    B, T, _ = qkv.shape
    y = _short_conv(qkv, conv_w)
    q = y[..., :DN_QK].reshape(B, T, DN_HEADS, DN_DK)
    k = y[..., DN_QK:2 * DN_QK].reshape(B, T, DN_HEADS, DN_DK)
    v = y[..., 2 * DN_QK:].reshape(B, T, DN_HEADS, DN_DV).astype(jnp.float32)
    q = _l2norm(q) * (DN_DK ** -0.5)
    k = _l2norm(k)
    tr = lambda a: jnp.transpose(a, (0, 2, 1, 3))
    beta = jax.nn.sigmoid(b_raw.astype(jnp.float32)).reshape(B, T, 2, DN_HEADS).transpose(2, 0, 3, 1)
    a = a_raw.astype(jnp.float32).reshape(B, T, 2, DN_HEADS).transpose(2, 0, 3, 1)
    g = -jnp.exp(a_log.astype(jnp.float32))[:, None, :, None] * jax.nn.softplus(
        a + dt_bias.astype(jnp.float32)[:, None, :, None])
    return tr(q), tr(k), tr(v), beta, g


def _gdn_chunked(q, k, v, g, beta, s0):
    B, H, T, Dk = q.shape
    Dv = v.shape[-1]
    C = DN_CHUNK
    n = T // C
    ch = lambda a: a.reshape(B, H, n, C, *a.shape[3:])
    q, k, v, g, beta = ch(q), ch(k), ch(v), ch(g), ch(beta)
    g = jnp.cumsum(g, axis=-1)
    incl = jnp.tril(jnp.ones((C, C), bool))
    strict = jnp.tril(jnp.ones((C, C), bool), -1)
    decay = jnp.exp(jnp.where(incl, g[..., :, None] - g[..., None, :], -jnp.inf))
    kb = k * beta[..., None]
    lmat = jnp.where(strict, jnp.einsum('bhncd,bhnsd->bhncs', kb, k) * decay, 0.0)
    eye = jnp.eye(C, dtype=jnp.float32)
    tinv = lax.linalg.triangular_solve(eye + lmat, jnp.broadcast_to(eye, lmat.shape),
                                       left_side=True, lower=True)
    u = tinv @ (v * beta[..., None])
    w = tinv @ (kb * jnp.exp(g)[..., None])
    a_intra = jnp.einsum('bhncd,bhnsd->bhncs', q, k) * decay
    q_g = q * jnp.exp(g)[..., None]
    g_last = g[..., -1]
    k_tail = k * jnp.exp(g_last[..., None] - g)[..., None]

    def step(S, xs):
        w_i, u_i, a_i, qg_i, kt_i, gl_i = xs
        v_new = u_i - jnp.einsum('bhck,bhkv->bhcv', w_i, S)
        o = jnp.einsum('bhck,bhkv->bhcv', qg_i, S) + jnp.einsum('bhcs,bhsv->bhcv', a_i, v_new)
        S = S * jnp.exp(gl_i)[..., None, None] + jnp.einsum('bhck,bhcv->bhkv', kt_i, v_new)
        return S, o

    mv = lambda a: jnp.moveaxis(a, 2, 0)
    s_fin, o = lax.scan(step, s0, (mv(w), mv(u), mv(a_intra), mv(q_g), mv(k_tail), mv(g_last)))
    o = jnp.moveaxis(o, 0, 2).reshape(B, H, T, Dv)
    return o, s_fin


def _gated_deltanet(qkv_l, z_l, b_l, a_l, qkv_c, z_c, b_c, a_c, conv_w, a_log, dt_bias, norm_g, need_ctx):
    ql, kl, vl, betal, gl = _dn_prep(qkv_l, b_l, a_l, conv_w, a_log, dt_bias)
    qc, kc, vc, betac, gc = _dn_prep(qkv_c, b_c, a_c, conv_w, a_log, dt_bias)
    B = ql.shape[0]
    s0 = jnp.zeros((B, DN_HEADS, DN_DK, DN_DV), jnp.float32)
    fl = lambda a: jnp.flip(a, axis=2)
    oc_f, sc_f = _gdn_chunked(qc, kc, vc, gc[0], betac[0], s0)
    ol_f, _ = _gdn_chunked(ql, kl, vl, gl[0], betal[0], sc_f)
    oc_b, sc_b = _gdn_chunked(fl(qc), fl(kc), fl(vc), fl(gc[1]), fl(betac[1]), s0)
    ol_b, _ = _gdn_chunked(fl(ql), fl(kl), fl(vl), fl(gl[1]), fl(betal[1]), sc_b)

    def finish(o, z):
        Bo, H, T, _ = o.shape
        o = jnp.transpose(o, (0, 2, 1, 3))
        zf = z.astype(jnp.float32).reshape(Bo, T, H, DN_DV)
        y = _rmsnorm(o, norm_g) * jax.nn.silu(zf)
        return y.reshape(Bo, T, DN_W).astype(z.dtype)

    out_l = finish(ol_f + fl(ol_b), z_l)
    out_c = finish(oc_f + fl(oc_b), z_c) if need_ctx else None
    return out_l, out_c


def _mixer(hl, hc, w_in, lam, subln, conv_w, a_log, dt_bias, dn_g, w_ba, w_bb, w_o, cos, sin, lam_init, need_ctx):
    pl = _split_cols(hl @ w_in)
    pc = _split_cols(hc @ w_in)
    da_l, da_c = _diff_attention(pl[0], pl[1], pl[2], pc[0], pc[1], pc[2], lam, subln, cos, sin,
                                 lam_init, need_ctx)
    dn_l, dn_c = _gated_deltanet(pl[3], pl[4], pl[5], pl[6], pc[3], pc[4], pc[5], pc[6],
                                 conv_w, a_log, dt_bias, dn_g, need_ctx)

    def merge(da, dn, p):
        y = jax.nn.sigmoid(p[7]) * (da @ w_ba) + jax.nn.sigmoid(p[8]) * (dn @ w_bb)
        return y @ w_o

    out_l = merge(da_l, dn_l, pl)
    out_c = merge(da_c, dn_c, pc) if need_ctx else None
    return out_l, out_c


def _peer(h, wq, keys, u_tab, v_tab):
    N, D = h.shape
    q = (h @ wq).reshape(N, PK_HEADS, 2, PK_HALF)
    s = jnp.einsum('nhpd,hpkd->nhpk', q, keys).astype(jnp.float32)
    top_s, top_i = lax.top_k(s, PK_TOPK)
    cand_s = (top_s[:, :, 0, :, None] + top_s[:, :, 1, None, :]).reshape(N, PK_HEADS, PK_TOPK * PK_TOPK)
    cand_i = (top_i[:, :, 0, :, None] * N_KEYS + top_i[:, :, 1, None, :]).reshape(N, PK_HEADS, PK_TOPK * PK_TOPK)
    best_s, best_pos = lax.top_k(cand_s, PK_TOPK)
    idx = jnp.take_along_axis(cand_i, best_pos, axis=-1)
    gate = jax.nn.softmax(best_s, axis=-1)
    nblk = N // TOK_BLOCK

    def block(args):
        hb, ib, gb = args
        ib = ib.reshape(TOK_BLOCK, PK_HEADS * PK_TOPK)
        gb = gb.reshape(TOK_BLOCK, PK_HEADS * PK_TOPK).astype(hb.dtype)
        u = jnp.take(u_tab, ib, axis=0)
        act = jax.nn.gelu(jnp.einsum('tnd,td->tn', u, hb), approximate=False)
        v = jnp.take(v_tab, ib, axis=0)
        return jnp.einsum('tn,tnd->td', gb * act, v)

    out = lax.map(block, (h.reshape(nblk, TOK_BLOCK, D),
                          idx.reshape(nblk, TOK_BLOCK, PK_HEADS, PK_TOPK),
                          gate.reshape(nblk, TOK_BLOCK, PK_HEADS, PK_TOPK)))
    return out.reshape(N, D)


def setup_inputs(seed: int = 0) -> dict:
    key = jax.random.key(seed)
    ks = jax.random.split(key, 24)
    D = D_MODEL
    nrm = lambda k, shape, s: jax.random.normal(k, shape, jnp.float32) * s
    dt = jnp.exp(jax.random.uniform(ks[13], (DEPTH, 2, DN_HEADS), jnp.float32,
                                    math.log(1e-3), math.log(1e-1)))
    return {
        'x': nrm(ks[0], (BATCH, SEQ, D), 1.0),
        'c': nrm(ks[1], (BATCH, D), 1.0),
        'ctx': nrm(ks[2], (BATCH, CTX_LEN, D), 1.0),
        'c_ctx': nrm(ks[3], (D,), 1.0),
        'w_ada': nrm(ks[4], (DEPTH, D, 6 * D), 0.5 * D ** -0.5),
        'b_ada': nrm(ks[5], (DEPTH, 6 * D), 0.02),
        'norm1_g': 1.0 + nrm(ks[6], (DEPTH, D), 0.02),
        'norm2_g': 1.0 + nrm(ks[7], (DEPTH, D), 0.02),
        'w_in': nrm(ks[8], (DEPTH, D, N_IN), D ** -0.5),
        'da_lambda': nrm(ks[9], (DEPTH, 4, DA_DIM), 0.1),
        'da_subln': 1.0 + nrm(ks[10], (DEPTH, DA_VDIM), 0.02),
        'dn_conv': nrm(ks[11], (DEPTH, DN_CONV, DN_QKV), DN_CONV ** -0.5),
        'dn_a_log': jnp.log(jax.random.uniform(ks[12], (DEPTH, 2, DN_HEADS), jnp.float32, 1.0, 16.0)),
        'dn_dt_bias': dt + jnp.log(-jnp.expm1(-dt)),
        'dn_norm': 1.0 + nrm(ks[14], (DEPTH, DN_DV), 0.02),
        'w_branch_a': nrm(ks[15], (DEPTH, DA_W, D), DA_W ** -0.5),
        'w_branch_b': nrm(ks[16], (DEPTH, DN_W, D), DN_W ** -0.5),
        'w_out': nrm(ks[17], (DEPTH, D, D), D ** -0.5),
        'peer_wq': nrm(ks[18], (DEPTH, D, PK_HEADS * PK_DQ), D ** -0.5),
        'peer_keys': nrm(ks[19], (DEPTH, PK_HEADS, 2, N_KEYS, PK_HALF), PK_HALF ** -0.5),
        'peer_u': nrm(ks[20], (DEPTH, N_EXPERTS, D), D ** -0.5),
        'peer_v': nrm(ks[21], (DEPTH, N_EXPERTS, D), PK_HEADS ** -0.5),
        'final_g': 1.0 + nrm(ks[22], (D,), 0.02),
    }


def reference(x, c, ctx, c_ctx, w_ada, b_ada, norm1_g, norm2_g, w_in, da_lambda, da_subln,
              dn_conv, dn_a_log, dn_dt_bias, dn_norm, w_branch_a, w_branch_b, w_out,
              peer_wq, peer_keys, peer_u, peer_v, final_g):
    B, S, D = x.shape
    n_rows = S // GRID_W
    cos, sin = _axial_rope(n_rows, DA_DIM)
    sc = jax.nn.silu(c)
    scc = jax.nn.silu(c_ctx)
    xl, xc = x, ctx
    for layer in range(DEPTH):
        need_ctx = layer < DEPTH - 1
        lam_init = 0.8 - 0.6 * math.exp(-0.3 * layer)
        mod_l = jnp.split((sc @ w_ada[layer] + b_ada[layer])[:, None, :], 6, axis=-1)
        mod_c = jnp.split((scc @ w_ada[layer] + b_ada[layer])[None, None, :], 6, axis=-1)
        hl = _modulate(xl, norm1_g[layer], mod_l[0], mod_l[1])
        hc = _modulate(xc, norm1_g[layer], mod_c[0], mod_c[1])
        yl, yc = _mixer(hl, hc, w_in[layer], da_lambda[layer], da_subln[layer], dn_conv[layer],
                        dn_a_log[layer], dn_dt_bias[layer], dn_norm[layer], w_branch_a[layer],
                        w_branch_b[layer], w_out[layer], cos, sin, lam_init, need_ctx)
        xl = xl + mod_l[2] * yl
        hl = _modulate(xl, norm2_g[layer], mod_l[3], mod_l[4])
        if need_ctx:
            xc = xc + mod_c[2] * yc
            hc = _modulate(xc, norm2_g[layer], mod_c[3], mod_c[4])
            tokens = jnp.concatenate([hl.reshape(-1, D), hc.reshape(-1, D)], axis=0)
            f = _peer(tokens, peer_wq[layer], peer_keys[layer], peer_u[layer], peer_v[layer])
            xl = xl + mod_l[5] * f[:B * S].reshape(B, S, D)
            xc = xc + mod_c[5] * f[B * S:].reshape(xc.shape)
        else:
            f = _peer(hl.reshape(-1, D), peer_wq[layer], peer_keys[layer], peer_u[layer], peer_v[layer])
            xl = xl + mod_l[5] * f.reshape(B, S, D)
    return _rmsnorm(xl, final_g)
```

```python
from concourse.bass_utils import run_bass_kernel_spmd
from contextlib import ExitStack
import numpy as np
import concourse.bass as bass
import concourse.mybir as mybir

F32 = mybir.dt.float32
BF16 = mybir.dt.bfloat16
U32 = mybir.dt.uint32
I32 = mybir.dt.int32
ALU = mybir.AluOpType
AF = mybir.ActivationFunctionType
AX = mybir.AxisListType


class Buf:
    __slots__ = ("name", "t", "lw", "rd")

    def __init__(self, name, t):
        self.name = name
        self.t = t
        self.lw = None
        self.rd = {}

    def __getitem__(self, idx):
        return V(self, self.t[idx])

    @property
    def ap(self):
        return self.t[:]


class V:
    __slots__ = ("b", "ap")

    def __init__(self, b, ap):
        self.b = b
        self.ap = ap

    def __getitem__(self, idx):
        return V(self.b, self.ap[idx])

    def re(self, pat, **kw):
        return V(self.b, self.ap.rearrange(pat, **kw))

    def bc(self, shape):
        return V(self.b, self.ap.to_broadcast(list(shape)))

    def us(self, axis):
        return V(self.b, self.ap.unsqueeze(axis))


class K:
    def __init__(self, nc, es, same_engine_sync=True, n_dma_sems=8):
        self.nc = nc
        self.es = es
        self.same = same_engine_sync
        self.engs = {"pe": nc.tensor, "dve": nc.vector, "act": nc.scalar, "pool": nc.gpsimd, "sp": nc.sync}
        self.sem = {}
        self.cnt = {}
        self.clock = {}
        self.hist = {}
        for k in ["pe", "dve", "act", "pool"]:
            self.sem[k] = es.enter_context(nc.semaphore("s_" + k))
            self.cnt[k] = 0
            self.hist[k] = [None]
        for k in self.engs:
            self.clock[k] = {}
        self.ndma = n_dma_sems
        for i in range(n_dma_sems):
            k = "q%d" % i
            self.sem[k] = es.enter_context(nc.semaphore("s_" + k))
            self.cnt[k] = 0
            self.hist[k] = [None]
        self.dma_rr = 0
        self.dmerged = {}
        self.nbuf = 0
        self.nwait = 0
        self.ninst = 0

    def sb(self, shape, dt=F32, name=None):
        self.nbuf += 1
        name = name or ("sb%d" % self.nbuf)
        t = self.es.enter_context(self.nc.sbuf_tensor(name, list(shape), dt))
        return Buf(name, t)

    def ps(self, shape, dt=F32, name=None):
        self.nbuf += 1
        name = name or ("ps%d" % self.nbuf)
        t = self.es.enter_context(self.nc.psum_tensor(name, list(shape), dt))
        return Buf(name, t)

    def dram(self, name, shape, dt=F32, kind="Internal"):
        t = self.nc.dram_tensor(name, list(shape), dt, kind=kind)
        return Buf(name, t.ap())

    def _merge(self, ek, other):
        c = self.clock[ek]
        for k, v in other.items():
            if c.get(k, 0) < v:
                c[k] = v

    def _need(self, ek, dep):
        if dep is None:
            return
        k2, n = dep
        if k2 == ek and (ek == "pe" or not self.same):
            return
        c = self.clock[ek]
        if c.get(k2, 0) >= n:
            return
        if k2[0] == "q":
            n = self.cnt[k2] * 16
            self.engs[ek].wait_ge(self.sem[k2], n)
            c[k2] = n // 16
            snaps = self.hist[k2]
            j0 = self.dmerged.get((ek, k2), 0)
            for j in range(j0 + 1, n // 16 + 1):
                self._merge(ek, snaps[j])
            self.dmerged[(ek, k2)] = n // 16
        else:
            self.engs[ek].wait_ge(self.sem[k2], n)
            c[k2] = n
            s = self.hist[k2][n]
            if s:
                self._merge(ek, s)
        self.nwait += 1

    def _deps(self, ek, reads, writes):
        for v in reads:
            b = v.b if isinstance(v, V) else v
            self._need(ek, b.lw)
        for v in writes:
            b = v.b if isinstance(v, V) else v
            self._need(ek, b.lw)
            for k2, n in list(b.rd.items()):
                self._need(ek, (k2, n))

    def _commit(self, key, n, reads, writes):
        for v in reads:
            b = v.b if isinstance(v, V) else v
            if b.rd.get(key, 0) < n:
                b.rd[key] = n
        for v in writes:
            b = v.b if isinstance(v, V) else v
            b.lw = (key, n)
            b.rd = {}

    def op(self, ek, fn, reads, writes):
        self._deps(ek, reads, writes)
        inst = fn()
        self.cnt[ek] += 1
        n = self.cnt[ek]
        inst.then_inc(self.sem[ek], 1)
        self.clock[ek][ek] = n if (ek == "pe" or not self.same) else self.clock[ek].get(ek, 0)
        self.hist[ek].append(dict(self.clock[ek]))
        self._commit(ek, n, reads, writes)
        self.ninst += 1
        return inst

    def dma(self, out, in_, q="sp", **kw):
        self._deps(q, [in_], [out])
        dk = "q%d" % self.dma_rr
        self.dma_rr = (self.dma_rr + 1) % self.ndma
        inst = self.engs[q].dma_start(out=out.ap, in_=in_.ap, **kw)
        inst.then_inc(self.sem[dk], 16)
        self.cnt[dk] += 1
        n = self.cnt[dk]
        self.hist[dk].append(dict(self.clock[q]))
        self._commit(dk, n, [in_], [out])
        self.ninst += 1
        return inst

    def barrier(self):
        for ek in self.engs:
            for k2 in self.sem:
                if k2 == ek:
                    if self.cnt[k2] > 0 and ek != "pe":
                        self._need(ek, (k2, self.cnt[k2]))
                    continue
                if self.cnt[k2] > 0:
                    self._need(ek, (k2, self.cnt[k2]))

    def finish(self, bufs, ek="sp"):
        for b in bufs:
            self._need(ek, b.lw)

    def mm(self, out, lhsT, rhs, start=True, stop=True, **kw):
        return self.op("pe", lambda: self.nc.tensor.matmul(out.ap, lhsT.ap, rhs.ap, start=start, stop=stop, **kw),
                       [lhsT, rhs], [out])

    def tr(self, out, in_, ident):
        return self.op("pe", lambda: self.nc.tensor.transpose(out.ap, in_.ap, ident.ap), [in_, ident], [out])

    def act(self, out, in_, func, bias=None, scale=None, accum=None):
        kw = {}
        rd = [in_]
        wr = [out]
        if bias is not None:
            if isinstance(bias, V):
                kw["bias"] = bias.ap
                rd.append(bias)
            else:
                kw["bias"] = bias
        if scale is not None:
            if isinstance(scale, V):
                kw["scale"] = scale.ap
                rd.append(scale)
            else:
                kw["scale"] = scale
        if accum is not None:
            kw["accum_out"] = accum.ap
            wr.append(accum)
        return self.op("act", lambda: self.nc.scalar.activation(out.ap, in_.ap, func, **kw), rd, wr)

    def tt(self, out, a, b, op, eng="dve"):
        e = self.engs[eng]
        return self.op(eng, lambda: e.tensor_tensor(out=out.ap, in0=a.ap, in1=b.ap, op=op), [a, b], [out])

    def ts(self, out, a, s1, op0, s2=None, op1=None, eng="dve", accum=None):
        e = self.engs[eng]
        rd = [a]
        wr = [out]
        kw = {}
        if isinstance(s1, V):
            rd.append(s1)
            s1 = s1.ap
        if isinstance(s2, V):
            rd.append(s2)
            s2 = s2.ap
        if op1 is not None:
            kw["op1"] = op1
        if accum is not None:
            kw["accum_out"] = accum.ap
            wr.append(accum)
        return self.op(eng, lambda: e.tensor_scalar(out=out.ap, in0=a.ap, scalar1=s1, scalar2=s2, op0=op0, **kw), rd, wr)

    def stt(self, out, a, s, b, op0, op1):
        rd = [a, b]
        if isinstance(s, V):
            rd.append(s)
            s = s.ap
        return self.op("dve", lambda: self.nc.vector.scalar_tensor_tensor(out=out.ap, in0=a.ap, scalar=s, in1=b.ap, op0=op0, op1=op1),
                       rd, [out])

    def copy(self, out, in_, eng="dve"):
        if eng == "act":
            return self.op("act", lambda: self.nc.scalar.copy(out.ap, in_.ap), [in_], [out])
        e = self.engs[eng]
        return self.op(eng, lambda: e.tensor_copy(out=out.ap, in_=in_.ap), [in_], [out])

    def memset(self, out, val, eng="dve"):
        e = self.engs[eng]
        return self.op(eng, lambda: e.memset(out.ap, val), [], [out])

    def reduce(self, out, in_, op, axis=AX.X, eng="dve"):
        e = self.engs[eng]
        return self.op(eng, lambda: e.tensor_reduce(out=out.ap, in_=in_.ap, op=op, axis=axis), [in_], [out])

    def recip(self, out, in_):
        return self.op("dve", lambda: self.nc.vector.reciprocal(out=out.ap, in_=in_.ap), [in_], [out])


T_A = 4352
NT_A = 34
EPS_A = 1e-6


def build_A(do_gdn=True):
    nc = bass.Bass("TRN2", target_bir_lowering=False)
    es = ExitStack()
    k = K(nc, es)
    D = 1024
    T = T_A
    xT = k.dram("xT", [D, T], F32, kind="ExternalInput")
    scT = k.dram("scT", [128, 8, 2], F32, kind="ExternalInput")
    w_ada01 = k.dram("w_ada01", [D, 2048], F32, kind="ExternalInput")
    b_adaT = k.dram("b_adaT", [128, 16], F32, kind="ExternalInput")
    g1T = k.dram("g1T", [128, 8], F32, kind="ExternalInput")
    w_att = k.dram("w_att", [D, 2560], F32, kind="ExternalInput")
    w_dn = k.dram("w_dn", [D, 2048], F32, kind="ExternalInput")
    w_ba16 = k.dram("w_ba16", [D, 16], F32, kind="ExternalInput")
    cosT = k.dram("cosT", [128, T], F32, kind="ExternalInput")
    sinT = k.dram("sinT", [128, T], F32, kind="ExternalInput")
    lamv = k.dram("lamv", [1, 4, 64], F32, kind="ExternalInput")
    laminit = k.dram("laminit", [1, 2], F32, kind="ExternalInput")
    subln = k.dram("subln", [128, 1], F32, kind="ExternalInput")
    cw = k.dram("cw", [128, 5, 12], F32, kind="ExternalInput")
    alog = k.dram("alog", [1, 8], F32, kind="ExternalInput")
    dtb = k.dram("dtb", [1, 8], F32, kind="ExternalInput")
    dnorm = k.dram("dnorm", [128, 1], F32, kind="ExternalInput")
    daT_o = k.dram("daT_o", [512, T], F32, kind="ExternalOutput")
    dnT_o = k.dram("dnT_o", [512, T], F32, kind="ExternalOutput")
    QK_d = k.dram("QK_d", [4, 2, 128, T], BF16)
    V_d = k.dram("V_d", [T, 512], BF16)
    DN_d = k.dram("DN_d", [4, 4, 128, T], F32)
    BA_d = k.dram("BA_d", [T, 16], F32)
    lam_d = k.dram("lam_d", [1, 2], F32)

    PS = [k.ps([128, 512], F32, name="psb%d" % i) for i in range(8)]
    ident = k.sb([128, 128], F32, name="ident")
    ones_f = k.sb([128, 128], F32, name="ones_f")
    ones_b = k.sb([128, 128], BF16, name="ones_b")
    k.memset(ones_f[:], 1.0)
    k.memset(ones_b[:], 1.0)
    k.memset(ident[:], 1.0)
    k.op("pool", lambda: nc.gpsimd.affine_select(out=ident[:].ap, in_=ident[:].ap, pattern=[[-1, 128]],
                                                 compare_op=ALU.is_equal, fill=0.0, base=0, channel_multiplier=1),
         [ident], [ident])
    msc = k.sb([128, 8, 2], F32, name="msc")
    msh = k.sb([128, 8, 2], F32, name="msh")
    lam_bc = k.sb([128, 2], F32, name="lam_bc")

    p0 = ExitStack()
    k.es = p0
    scs = k.sb([128, 8, 2], F32)
    wst = [k.sb([128, 8, 512], F32) for _ in range(2)]
    bT = k.sb([128, 16], F32); gT = k.sb([128, 8], F32); modT = k.sb([128, 16, 2], F32)
    k.dma(scs[:], scT[:]); k.act(scs[:], scs[:], AF.Silu)
    k.dma(bT[:], b_adaT[:]); k.dma(gT[:], g1T[:])
    for q4 in range(4):
        w_ = wst[q4 % 2]
        k.dma(w_[:], V(w_ada01, w_ada01.t[:, q4 * 512:(q4 + 1) * 512].rearrange("(c p) j -> p c j", p=128)))
        for jl in range(4):
            jc = q4 * 4 + jl
            for dc in range(8):
                k.mm(PS[0][:, jc * 2:(jc + 1) * 2], w_[:, dc, jl * 128:(jl + 1) * 128], scs[:, dc, :], start=(dc == 0), stop=(dc == 7))
    k.tt(modT[:], PS[0][:, 0:32].re("p (j c) -> p j c", c=2), bT[:].us(2).bc([128, 16, 2]), ALU.add)
    k.copy(msh[:], modT[:, 0:8, :])
    k.stt(msc[:], modT[:, 8:16, :], 1.0, gT[:].us(2).bc([128, 8, 2]), ALU.add, ALU.mult)
    lv = k.sb([1, 4, 64], F32); lp = k.sb([1, 2, 64], F32); ls = k.sb([1, 2], F32); li = k.sb([1, 2], F32); lo = k.sb([1, 2], F32)
    sl = k.sb([128, 1], F32); lib = k.sb([128, 2], F32)
    k.dma(lv[:], lamv[:]); k.dma(li[:], laminit[:])
    lv4 = lv[:].re("o (a b) d -> o a b d", b=2)
    k.tt(lp[:], lv4[:, :, 0, :], lv4[:, :, 1, :], ALU.mult)
    k.reduce(ls[:], lp[:], ALU.add)
    k.act(ls[:], ls[:], AF.Exp)
    k.tt(lo[:, 0:1], ls[:, 1:2], ls[:, 0:1], ALU.subtract)
    k.tt(lo[:, 0:1], lo[:, 0:1], li[:, 0:1], ALU.subtract)
    k.copy(lo[:, 1:2], li[:, 1:2])
    k.dma(lam_d[:], lo[:])
    k.dma(lib[:], V(lam_d, lam_d.t[0:1, :].to_broadcast([128, 2])))
    k.dma(sl[:], subln[:])
    k.copy(lam_bc[:, 0:1], lib[:, 0:1])
    k.tt(lam_bc[:, 1:2], lib[:, 1:2], sl[:], ALU.mult)
    k.barrier()
    p0.close()

    p1 = ExitStack()
    k.es = p1
    watt = k.sb([128, 8, 2560], BF16); wdn = k.sb([128, 8, 2048], BF16); wba = k.sb([128, 8, 16], BF16)
    stw = k.sb([128, 8, 512], F32)
    for i in range(5):
        k.dma(stw[:], V(w_att, w_att.t[:, i * 512:(i + 1) * 512].rearrange("(c p) j -> p c j", p=128)))
        k.copy(watt[:, :, i * 512:(i + 1) * 512], stw[:], eng="pool")
    for i in range(4):
        k.dma(stw[:], V(w_dn, w_dn.t[:, i * 512:(i + 1) * 512].rearrange("(c p) j -> p c j", p=128)))
        k.copy(wdn[:, :, i * 512:(i + 1) * 512], stw[:], eng="pool")
    k.dma(stw[:, :, 0:16], V(w_ba16, w_ba16.t.rearrange("(c p) j -> p c j", p=128)))
    k.copy(wba[:], stw[:, :, 0:16], eng="pool")
    xb = k.sb([128, 8, 512], F32); sq = k.sb([128, 8, 512], F32)
    rstd = k.sb([128, 512], F32); tmp = k.sb([128, 512], F32)
    hT = k.sb([128, 8, 512], BF16)
    cosb = k.sb([128, 512], F32); sinb = k.sb([128, 512], F32)
    t1 = k.sb([128, 512], F32); t2 = k.sb([128, 512], F32)
    stb = [k.sb([128, 512], BF16) for _ in range(3)]
    stf = [k.sb([128, 512], F32) for _ in range(3)]
    stv = [k.sb([128, 512], BF16) for _ in range(2)]
    stba = [k.sb([128, 16], F32) for _ in range(2)]
    blocks = [(0, 256, 1)] + [(256 + 512 * i, 512, 0) for i in range(8)]
    cnt = 0
    for (n0, N, col) in blocks:
        k.dma(xb[:, :, :N], V(xT, xT.t[:, n0:n0 + N].rearrange("(c p) n -> p c n", p=128)))
        k.dma(cosb[:, :N], cosT[:, n0:n0 + N]); k.dma(sinb[:, :N], sinT[:, n0:n0 + N])
        k.act(sq[:, :, :N], xb[:, :, :N], AF.Square)
        for dc in range(8):
            k.mm(PS[0][:, :N], ones_f[:], sq[:, dc, :N], start=(dc == 0), stop=(dc == 7))
        k.ts(rstd[:, :N], PS[0][:, :N], 1.0 / 1024, ALU.mult, EPS_A, ALU.add)
        k.act(rstd[:, :N], rstd[:, :N], AF.Sqrt)
        k.recip(rstd[:, :N], rstd[:, :N])
        for dc in range(8):
            k.tt(tmp[:, :N], xb[:, dc, :N], rstd[:, :N], ALU.mult)
            k.ts(hT[:, dc, :N], tmp[:, :N], msc[:, dc, col:col + 1], ALU.mult, msh[:, dc, col:col + 1], ALU.add)
        for h in range(4):
            for qk in range(2):
                base = qk * 1024
                pr = PS[1 + (cnt % 2) * 2]; pw = PS[2 + (cnt % 2) * 2]
                for dc in range(8):
                    k.mm(pr[:, :N], watt[:, dc, base + h * 128: base + (h + 1) * 128], hT[:, dc, :N], start=(dc == 0), stop=(dc == 7))
                for dc in range(8):
                    k.mm(pw[:, :N], watt[:, dc, base + 512 + h * 128: base + 512 + (h + 1) * 128], hT[:, dc, :N], start=(dc == 0), stop=(dc == 7))
                k.tt(t1[:, :N], pr[:, :N], cosb[:, :N], ALU.mult)
                k.tt(t2[:, :N], pw[:, :N], sinb[:, :N], ALU.mult)
                sb_ = stb[cnt % 3]
                k.tt(sb_[:, :N], t1[:, :N], t2[:, :N], ALU.add, eng="pool")
                k.dma(V(QK_d, QK_d.t[h, qk, :, n0:n0 + N]), sb_[:, :N])
                cnt += 1
        for which in range(4):
            for h in range(4):
                pr = PS[5 + cnt % 2]
                c0 = which * 512 + h * 128
                for dc in range(8):
                    k.mm(pr[:, :N], wdn[:, dc, c0:c0 + 128], hT[:, dc, :N], start=(dc == 0), stop=(dc == 7))
                sf = stf[cnt % 3]
                k.copy(sf[:, :N], pr[:, :N], eng=("act" if cnt % 2 else "dve"))
                k.dma(V(DN_d, DN_d.t[which, h, :, n0:n0 + N]), sf[:, :N])
                cnt += 1
        for ti in range(N // 128):
            tsl = slice(ti * 128, (ti + 1) * 128)
            for dc in range(8):
                k.mm(PS[7][:], hT[:, dc, tsl], watt[:, dc, 2048:2560], start=(dc == 0), stop=(dc == 7))
            sv = stv[ti % 2]
            k.copy(sv[:], PS[7][:], eng="act")
            k.dma(V_d[n0 + ti * 128:n0 + (ti + 1) * 128, :], sv[:])
            for dc in range(8):
                k.mm(PS[0][:, 0:16], hT[:, dc, tsl], wba[:, dc, :], start=(dc == 0), stop=(dc == 7))
            sba = stba[ti % 2]
            k.copy(sba[:], PS[0][:, 0:16])
            k.dma(BA_d[n0 + ti * 128:n0 + (ti + 1) * 128, :], sba[:])
    k.barrier()
    p1.close()

    p2 = ExitStack()
    k.es = p2
    qT = k.sb([128, T], BF16); kT = k.sb([128, T], BF16); vt = k.sb([128, NT_A, 128], BF16)
    pT = [k.sb([128, 512], BF16) for _ in range(3)]
    rz = k.sb([128, 512], F32); o0 = k.sb([128, 512], F32); o1 = k.sb([128, 512], F32)
    asq = k.sb([128, 512], F32); ar = k.sb([128, 512], F32); outb = [k.sb([128, 512], F32) for _ in range(2)]
    qblocks = [(0, 256, 2)] + [(256 + 512 * i, 512, NT_A) for i in range(8)]
    pc = 0
    for h in range(4):
        k.dma(qT[:], V(QK_d, QK_d.t[h, 0]))
        k.dma(kT[:], V(QK_d, QK_d.t[h, 1]))
        k.dma(vt[:], V(V_d, V_d.t[:, h * 128:(h + 1) * 128].rearrange("(j p) e -> p j e", p=128)))
        for bi, (n0, N, nk) in enumerate(qblocks):
            for c in range(2):
                rows = slice(c * 64, (c + 1) * 64)
                po = PS[4 + c]; pz = PS[6 + c]
                for j in range(nk):
                    pss = PS[pc % 3]
                    k.mm(pss[:, :N], kT[rows, j * 128:(j + 1) * 128], qT[rows, n0:n0 + N])
                    p_ = pT[pc % 3]
                    k.act(p_[:, :N], pss[:, :N], AF.Exp, scale=0.125)
                    k.mm(po[:, :N], vt[:, j, :], p_[:, :N], start=(j == 0), stop=(j == nk - 1))
                    k.mm(pz[:, :N], ones_b[:], p_[:, :N], start=(j == 0), stop=(j == nk - 1))
                    pc += 1
            k.recip(rz[:, :N], PS[6][:, :N])
            k.tt(o0[:, :N], PS[4][:, :N], rz[:, :N], ALU.mult)
            k.recip(rz[:, :N], PS[7][:, :N])
            k.tt(o1[:, :N], PS[5][:, :N], rz[:, :N], ALU.mult)
            k.stt(o0[:, :N], o1[:, :N], lam_bc[:, 0:1], o0[:, :N], ALU.mult, ALU.add)
            k.act(asq[:, :N], o0[:, :N], AF.Square)
            k.mm(PS[3][:, :N], ones_f[:], asq[:, :N])
            k.ts(ar[:, :N], PS[3][:, :N], 1.0 / 128, ALU.mult, EPS_A, ALU.add)
            k.act(ar[:, :N], ar[:, :N], AF.Sqrt)
            k.recip(ar[:, :N], ar[:, :N])
            ob = outb[bi % 2]
            k.stt(ob[:, :N], o0[:, :N], lam_bc[:, 1:2], ar[:, :N], ALU.mult, ALU.mult)
            k.dma(daT_o[h * 128:(h + 1) * 128, n0:n0 + N], ob[:, :N])
    k.barrier()
    p2.close()
    if do_gdn:
        build_gdn(nc, k, PS, ident, ones_f, DN_d, BA_d, cw, alog, dtb, dnorm, dnT_o)
    k.barrier()
    k.finish([daT_o, dnT_o] if do_gdn else [daT_o])
    print("A: insts", k.ninst, "waits", k.nwait, dict(k.cnt))
    return nc, es


def build_gdn(nc, k, PS, ident, ones_f, DN_d, BA_d, cw, alog, dtb, dnorm, dnT_o):
    T = T_A
    GQ_d = k.dram("GQ_d", [4, 128, T], BF16)
    GK_d = k.dram("GK_d", [4, 128, T], BF16)
    GKt_d = k.dram("GKt_d", [4, T, 128], BF16)
    GVt_d = k.dram("GVt_d", [4, T, 128], BF16)
    O_d = k.dram("O_d", [2, T, 512], F32)

    pa = ExitStack()
    k.es = pa
    cws = k.sb([128, 5, 12], F32)
    k.dma(cws[:], cw[:])
    raw = k.sb([128, T], F32); y = k.sb([128, T], F32); ys = k.sb([128, T], F32)
    sqb = k.sb([128, 512], F32); rs = k.sb([128, 512], F32)
    obf = k.sb([128, T], BF16)
    tkb = [k.sb([128, 4, 128], BF16) for _ in range(2)]
    segs = [(0, 256), (256, T)]
    blocks = [(0, 256)] + [(256 + 512 * i, 512) for i in range(8)]
    for h in range(4):
        for which in range(3):
            wi = which * 4 + h
            k.dma(raw[:], V(DN_d, DN_d.t[which, h]))
            k.ts(y[:], raw[:], cws[:, 2, wi:wi + 1], ALU.mult)
            for j in (0, 1, 3, 4):
                off = j - 2
                for (s0, s1) in segs:
                    lo = max(s0, s0 - off); hi = min(s1, s1 - off)
                    k.stt(y[:, lo:hi], raw[:, lo + off:hi + off], cws[:, j, wi:wi + 1], y[:, lo:hi], ALU.mult, ALU.add)
            k.act(ys[:], y[:], AF.Silu)
            if which < 2:
                for (n0, N) in blocks:
                    k.act(sqb[:, :N], ys[:, n0:n0 + N], AF.Square)
                    k.mm(PS[0][:, :N], ones_f[:], sqb[:, :N])
                    k.ts(rs[:, :N], PS[0][:, :N], EPS_A, ALU.add)
                    k.act(rs[:, :N], rs[:, :N], AF.Sqrt)
                    k.recip(rs[:, :N], rs[:, :N])
                    if which == 0:
                        k.stt(ys[:, n0:n0 + N], ys[:, n0:n0 + N], 128.0 ** -0.5, rs[:, :N], ALU.mult, ALU.mult)
                    else:
                        k.tt(ys[:, n0:n0 + N], ys[:, n0:n0 + N], rs[:, :N], ALU.mult)
                k.copy(obf[:], ys[:], eng="pool")
                k.dma(V((GQ_d, GK_d)[which], (GQ_d, GK_d)[which].t[h]), obf[:])
            if which >= 1:
                dstd = (GKt_d, GVt_d)[which - 1]
                for g4 in range(9):
                    tiles = list(range(g4 * 4, min(g4 * 4 + 4, NT_A)))
                    pb = PS[1 + g4 % 2]
                    for q, tl in enumerate(tiles):
                        k.tr(pb[:, q * 128:(q + 1) * 128], ys[:, tl * 128:(tl + 1) * 128], ident[:])
                    tb = tkb[g4 % 2]
                    nq = len(tiles)
                    k.copy(tb[:, :nq, :], pb[:, :nq * 128].re("p (c n) -> p c n", c=nq), eng=("act" if g4 % 2 else "dve"))
                    t0_ = tiles[0] * 128
                    k.dma(V(dstd, dstd.t[h, t0_:t0_ + nq * 128, :].rearrange("(c p) d -> p c d", p=128)), tb[:, :nq, :])
    k.barrier()
    pa.close()

    pb_ = ExitStack()
    k.es = pb_
    BA = k.sb([128, NT_A, 16], F32); beta = k.sb([128, NT_A, 8], F32); nbeta = k.sb([128, NT_A, 8], F32)
    gg = k.sb([128, NT_A, 8], F32); tmpg = k.sb([128, NT_A, 8], F32)
    alb = k.sb([128, 8], F32); dtbb = k.sb([128, 8], F32)
    k.dma(BA[:], V(BA_d, BA_d.t.rearrange("(j p) c -> p j c", p=128)))
    k.dma(alb[:], V(alog, alog.t[0:1, :].to_broadcast([128, 8])))
    k.dma(dtbb[:], V(dtb, dtb.t[0:1, :].to_broadcast([128, 8])))
    k.act(beta[:], BA[:, :, 0:8], AF.Sigmoid)
    k.ts(nbeta[:], beta[:], -1.0, ALU.mult)
    k.act(alb[:], alb[:], AF.Exp)
    k.ts(alb[:], alb[:], -1.0, ALU.mult)
    k.tt(tmpg[:], BA[:, :, 8:16], dtbb[:].us(1).bc([128, NT_A, 8]), ALU.add)
    k.act(tmpg[:], tmpg[:], AF.Exp)
    k.act(tmpg[:], tmpg[:], AF.Ln, bias=1.0)
    k.tt(gg[:], tmpg[:], alb[:].us(1).bc([128, NT_A, 8]), ALU.mult)

    def tri(name, pattern, cm, base, zr, zc):
        m = k.sb([128, 128], F32, name=name)
        k.memset(m[:], 1.0)
        k.op("pool", lambda: nc.gpsimd.affine_select(out=m[:].ap, in_=m[:].ap, pattern=pattern, compare_op=ALU.is_ge,
                                                     fill=0.0, base=base, channel_multiplier=cm), [m], [m])
        k.memset(m[zr, zc], 0.0)
        return m
    lo_r, hi_r = slice(64, 128), slice(0, 64)
    LTi = tri("LTi", [[-1, 128]], 1, 0, slice(64, 128), slice(0, 64))
    LTs = tri("LTs", [[-1, 128]], 1, -1, slice(64, 128), slice(0, 64))
    UTi = tri("UTi", [[1, 128]], -1, 0, slice(0, 64), slice(64, 128))
    UTs = tri("UTs", [[1, 128]], -1, -1, slice(0, 64), slice(64, 128))
    blk1 = k.sb([128, 128], F32, name="blk1")
    k.memset(blk1[:], 0.0); k.memset(blk1[0:64, 0:64], 1.0); k.memset(blk1[64:128, 64:128], 1.0)

    def negmask(m, name):
        n = k.sb([128, 128], F32, name=name)
        k.ts(n[:], m[:], 30000.0, ALU.mult, -30000.0, ALU.add)
        return n
    NEG = {id(LTi): negmask(LTi, "nLTi"), id(LTs): negmask(LTs, "nLTs"), id(UTi): negmask(UTi, "nUTi"), id(UTs): negmask(UTs, "nUTs")}
    dirm = [dict(Mi=LTi, Ms=LTs, MiT=UTi, MsT=UTs), dict(Mi=UTi, Ms=UTs, MiT=LTi, MsT=LTs)]

    quart = [[PS[p][:, q * 128:(q + 1) * 128] for q in range(4)] for p in range(8)]
    qi = [0] * 8

    def pq(p):
        b = quart[p][qi[p] % 4]
        qi[p] += 1
        return b

    pairs = [(d, h) for d in range(2) for h in range(4)]
    NP = 8
    mk = lambda dt_, nm: [k.sb([128, 128], dt_, name="%s%d" % (nm, p)) for p in range(NP)]
    R1 = mk(F32, "R1"); R2 = mk(F32, "R2"); Dst = mk(F32, "Dst"); DT = mk(F32, "DT"); Egc = mk(F32, "Egc")
    Xa = mk(F32, "Xa"); XTa = mk(F32, "XTa"); Xb = mk(F32, "Xb"); XTb = mk(F32, "XTb"); PT = mk(F32, "PT")
    tinvT = mk(BF16, "tinvT"); vb = mk(BF16, "vb"); kbg = mk(BF16, "kbg"); kt0 = mk(BF16, "kt0"); kt1 = mk(BF16, "kt1")
    u_sb = mk(F32, "u_sb"); wT_sb = mk(BF16, "wT_sb"); aT = mk(BF16, "aT"); qgT = mk(BF16, "qgT"); vn = mk(BF16, "vn")
    o_sb = mk(F32, "o_sb"); S = mk(F32, "S"); S_bf = mk(BF16, "S_bf")
    gcs = [k.sb([128, 4], F32, name="gcs%d" % d) for d in range(2)]
    gls = [k.sb([128, 4], F32, name="gls%d" % d) for d in range(2)]
    egc = [k.sb([128, 4], F32, name="egc%d" % d) for d in range(2)]
    ekt = [k.sb([128, 4], F32, name="ekt%d" % d) for d in range(2)]
    bgc = [k.sb([128, 4], F32, name="bgc%d" % d) for d in range(2)]
    ld = [[k.sb([128, 4, 128], BF16, name="ld%d_%d" % (d, kind)) for kind in range(4)] for d in range(2)]
    for p in range(NP):
        k.memset(S[p][:], 0.0); k.memset(S_bf[p][:], 0.0); k.memset(vn[p][:], 0.0)
        k.memset(kt0[p][:], 0.0); k.memset(kt1[p][:], 0.0)
    Ftiles = list(range(NT_A))
    Btiles = [1, 0] + list(range(NT_A - 1, 1, -1))
    for step in range(NT_A):
        tau = [Ftiles[step], Btiles[step]]
        for d in range(2):
            tsl = slice(tau[d] * 128, (tau[d] + 1) * 128)
            k.dma(ld[d][0][:], V(GQ_d, GQ_d.t[:, :, tsl].rearrange("h p n -> p h n")))
            k.dma(ld[d][1][:], V(GK_d, GK_d.t[:, :, tsl].rearrange("h p n -> p h n")))
            k.dma(ld[d][2][:], V(GKt_d, GKt_d.t[:, tsl, :].rearrange("h p n -> p h n")))
            k.dma(ld[d][3][:], V(GVt_d, GVt_d.t[:, tsl, :].rearrange("h p n -> p h n")))
            gsl = gg[:, tau[d], d * 4:(d + 1) * 4]
            pg = pq(d)
            k.mm(pg[:, 0:4], dirm[d]["MiT"][:], gsl)
            k.copy(gcs[d][:], pg[:, 0:4])
            pg2 = pq(d)
            k.mm(pg2[:, 0:4], blk1[:], gsl)
            k.tt(gls[d][:], pg2[:, 0:4], gcs[d][:], ALU.subtract)
            k.act(egc[d][:], gcs[d][:], AF.Exp)
            k.act(ekt[d][:], gls[d][:], AF.Exp)
            k.tt(bgc[d][:], egc[d][:], beta[:, tau[d], d * 4:(d + 1) * 4], ALU.mult)
        kk_ps = [None] * NP; kq_ps = [None] * NP
        for p, (d, h) in enumerate(pairs):
            M = dirm[d]
            gcol = gg[:, tau[d], d * 4 + h:d * 4 + h + 1]
            qTt = ld[d][0][:, h, :]; kTt = ld[d][1][:, h, :]; ktok = ld[d][2][:, h, :]; vtok = ld[d][3][:, h, :]
            k.ts(R1[p][:], M["Ms"][:], gcol, ALU.mult)
            k.ts(R2[p][:], M["MiT"][:], gcol, ALU.mult, eng="pool")
            pG = pq(p)
            k.mm(pG[:], M["MiT"][:], R1[p][:], start=True, stop=False)
            k.mm(pG[:], ident[:], NEG[id(M["Ms"])][:], start=False, stop=True)
            k.act(Dst[p][:], pG[:], AF.Exp)
            pGT = pq(p)
            k.mm(pGT[:], M["Ms"][:], R2[p][:], start=True, stop=False)
            k.mm(pGT[:], ident[:], NEG[id(M["MiT"])][:], start=False, stop=True)
            k.act(DT[p][:], pGT[:], AF.Exp)
            pE = pq(p)
            k.mm(pE[:], ones_f[:], R2[p][:])
            k.act(Egc[p][:], pE[:], AF.Exp)
            pK = pq(p)
            k.mm(pK[:], kTt, kTt)
            k.stt(Xa[p][:], pK[:], nbeta[:, tau[d], d * 4 + h:d * 4 + h + 1], Dst[p][:], ALU.mult, ALU.mult)
            pKQ = pq(p)
            k.mm(pKQ[:], kTt, qTt)
            k.tt(aT[p][:], pKQ[:], DT[p][:], ALU.mult)
            k.tt(qgT[p][:], qTt, Egc[p][:], ALU.mult, eng="pool")
            bcol = beta[:, tau[d], d * 4 + h:d * 4 + h + 1]
            k.ts(vb[p][:], vtok, bcol, ALU.mult, eng="pool")
            k.ts(kbg[p][:], ktok, bgc[d][:, h:h + 1], ALU.mult, eng="pool")
            k.ts(kt0[p][0:64, :], ld[d][2][0:64, h, :], ekt[d][0:64, h:h + 1], ALU.mult)
            k.ts(kt1[p][64:128, :], ld[d][2][64:128, h, :], ekt[d][64:128, h:h + 1], ALU.mult)
            pX = pq(p)
            k.tr(pX[:], Xa[p][:], ident[:])
            k.copy(XTa[p][:], pX[:], eng="act")
            k.tt(PT[p][:], pX[:], ident[:], ALU.add)
        Xc, XTc, Xn, XTn = Xa, XTa, Xb, XTb
        for lev in range(1, 6):
            for p in range(NP):
                p1_ = pq(p)
                k.mm(p1_[:], XTc[p][:], Xc[p][:])
                k.copy(Xn[p][:], p1_[:], eng="act")
                if lev < 5:
                    p2_ = pq(p)
                    k.mm(p2_[:], Xc[p][:], XTc[p][:])
                    k.copy(XTn[p][:], p2_[:], eng="dve")
            for p in range(NP):
                p3_ = pq(p)
                k.mm(p3_[:], Xn[p][:], PT[p][:])
                k.tt(PT[p][:], p3_[:], PT[p][:], ALU.add)
            Xc, XTc, Xn, XTn = Xn, XTn, Xc, XTc
        for p in range(NP):
            k.copy(tinvT[p][:], PT[p][:], eng="pool")
            pu = pq(p)
            k.mm(pu[:], tinvT[p][:], vb[p][:])
            k.copy(u_sb[p][:], pu[:], eng="act")
            pw_ = pq(p)
            k.mm(pw_[:], kbg[p][:], tinvT[p][:])
            k.copy(wT_sb[p][:], pw_[:], eng="dve")
        for ci in range(2):
            for p, (d, h) in enumerate(pairs):
                c = ci if d == 0 else 1 - ci
                cs = slice(c * 64, (c + 1) * 64)
                last = (c * 64 + 63) if d == 0 else (c * 64)
                ktc = (kt0, kt1)[c][p]
                pv = pq(p)
                k.mm(pv[:], wT_sb[p][:], S_bf[p][:])
                k.tt(vn[p][cs, :], u_sb[p][cs, :], pv[cs, :], ALU.subtract)
                po = pq(p)
                k.mm(po[:], qgT[p][:], S_bf[p][:], start=True, stop=False)
                k.mm(po[:], aT[p][:], vn[p][:], start=False, stop=True)
                k.copy(o_sb[p][cs, :], po[cs, :], eng="act")
                ps_ = pq(p)
                k.mm(ps_[:], ktc[:], vn[p][:])
                k.stt(S[p][:], S[p][:], Egc[p][:, last:last + 1], ps_[:], ALU.mult, ALU.add)
                k.copy(S_bf[p][:], S[p][:], eng="pool")
        for p, (d, h) in enumerate(pairs):
            k.dma(O_d[d, tau[d] * 128:(tau[d] + 1) * 128, h * 128:(h + 1) * 128], o_sb[p][:])
    k.barrier()
    pb_.close()

    pd_ = ExitStack()
    k.es = pd_
    dn_s = k.sb([128, 1], F32)
    k.dma(dn_s[:], dnorm[:])
    of = [k.sb([128, 512], F32) for _ in range(2)]; ob_ = [k.sb([128, 512], F32) for _ in range(2)]
    oT = k.sb([128, 512], F32); sq2 = k.sb([128, 512], F32); rs2 = k.sb([128, 512], F32)
    zt = [k.sb([128, 4, 128], F32) for _ in range(2)]
    res_ = [k.sb([128, 4, 128], F32) for _ in range(2)]
    for tl in range(NT_A):
        tsl = slice(tl * 128, (tl + 1) * 128)
        a_, b_ = of[tl % 2], ob_[tl % 2]
        k.dma(a_[:], O_d[0, tsl, :]); k.dma(b_[:], O_d[1, tsl, :])
        k.dma(zt[tl % 2][:], V(DN_d, DN_d.t[3, :, :, tsl].rearrange("h p n -> p h n")))
        k.tt(a_[:], a_[:], b_[:], ALU.add)
        pbk = PS[tl % 2]
        for h in range(4):
            k.tr(pbk[:, h * 128:(h + 1) * 128], a_[:, h * 128:(h + 1) * 128], ident[:])
        k.copy(oT[:], pbk[:], eng="act")
        k.act(sq2[:], oT[:], AF.Square)
        k.mm(PS[2 + tl % 2][:], ones_f[:], sq2[:])
        k.ts(rs2[:], PS[2 + tl % 2][:], 1.0 / 128, ALU.mult, EPS_A, ALU.add)
        k.act(rs2[:], rs2[:], AF.Sqrt)
        k.recip(rs2[:], rs2[:])
        k.stt(oT[:], oT[:], dn_s[:], rs2[:], ALU.mult, ALU.mult)
        k.act(zt[tl % 2][:], zt[tl % 2][:], AF.Silu)
        r_ = res_[tl % 2]
        k.tt(r_[:], oT[:].re("p (h n) -> p h n", h=4), zt[tl % 2][:], ALU.mult)
        k.dma(V(dnT_o, dnT_o.t[:, tsl].rearrange("(h p) n -> p h n", p=128)), r_[:])
    k.barrier()
    pd_.close()


NTOK_B = 2176
NT_B = 17
EPS_ = 1e-6


def build_B():
    nc = bass.Bass("TRN2", target_bir_lowering=False)
    es = ExitStack()
    k = K(nc, es)
    D = 1024
    x_tok = k.dram("x_tok", [NTOK_B, D], F32, kind="ExternalInput")
    daT = k.dram("daT", [D, NTOK_B], F32, kind="ExternalInput")
    dnT = k.dram("dnT", [D, NTOK_B], F32, kind="ExternalInput")
    scT = k.dram("scT", [128, 8, 2], F32, kind="ExternalInput")
    w_ada = k.dram("w_ada", [D, 6144], F32, kind="ExternalInput")
    b_ada = k.dram("b_ada", [1, 6144], F32, kind="ExternalInput")
    g1 = k.dram("g1", [1, D], F32, kind="ExternalInput")
    g2 = k.dram("g2", [1, D], F32, kind="ExternalInput")
    fg = k.dram("fg", [1, D], F32, kind="ExternalInput")
    w_g = k.dram("w_g", [D, 2048], F32, kind="ExternalInput")
    w_ba = k.dram("w_ba", [D, D], F32, kind="ExternalInput")
    w_bb = k.dram("w_bb", [D, D], F32, kind="ExternalInput")
    w_o = k.dram("w_o", [D, D], F32, kind="ExternalInput")
    wq = k.dram("wq", [D, 2048], F32, kind="ExternalInput")
    keysT = k.dram("keysT", [128, 16, 128], F32, kind="ExternalInput")
    uT = k.dram("uT", [128, D, 128], F32, kind="ExternalInput")
    vP = k.dram("vP", [128, 128, D], F32, kind="ExternalInput")
    x2_o = k.dram("x2_o", [NTOK_B, D], F32, kind="ExternalOutput")
    fin_o = k.dram("fin_o", [NTOK_B, D], F32, kind="ExternalOutput")
    mod_d = k.dram("mod_d", [2, 6144], F32)
    x1_d = k.dram("x1_d", [NTOK_B, D], F32)

    def dr(b, ap):
        return V(b, ap)

    PS = [k.ps([128, 512], F32, name="psb%d" % i) for i in range(8)]
    ident = k.sb([128, 128], F32, name="ident")
    iotaI = k.sb([128, 128], F32, name="iotaI")
    iota16 = k.sb([128, 16], F32, name="iota16")
    h2T = k.sb([128, 8, NTOK_B], BF16, name="h2T")
    idx1T = k.sb([128, NTOK_B], BF16, name="idx1T")
    idx2T = k.sb([128, NTOK_B], BF16, name="idx2T")
    gateT = k.sb([128, NTOK_B], BF16, name="gateT")
    r_gt2 = [k.sb([128, D], F32, name="r_gt2_%d" % i) for i in range(2)]
    r_fg = k.sb([128, D], F32, name="r_fg")
    junk = k.sb([128, D], F32, name="junk")
    small = [[k.sb([128, 1], F32, name="sm%d_%d" % (i, j)) for j in range(4)] for i in range(4)]
    sm_i = [0]

    k.memset(ident[:], 1.0)
    k.op("pool", lambda: nc.gpsimd.affine_select(out=ident[:].ap, in_=ident[:].ap, pattern=[[-1, 128]],
                                                 compare_op=ALU.is_equal, fill=0.0, base=0, channel_multiplier=1),
         [ident], [ident])
    k.op("pool", lambda: nc.gpsimd.iota(iotaI[:].ap, [[1, 128]], base=0, channel_multiplier=0,
                                        allow_small_or_imprecise_dtypes=True), [], [iotaI])
    k.op("pool", lambda: nc.gpsimd.iota(iota16[:].ap, [[1, 16]], base=0, channel_multiplier=0,
                                        allow_small_or_imprecise_dtypes=True), [], [iota16])

    def norm_stats(xv):
        s = small[sm_i[0] % 4]
        sm_i[0] += 1
        k.act(junk[:], xv, AF.Square, accum=s[0][:])
        k.ts(s[1][:], s[0][:], 1.0 / 1024, ALU.mult, EPS_, ALU.add)
        k.act(s[2][:], s[1][:], AF.Sqrt)
        k.recip(s[3][:], s[2][:])
        return s[3][:]

    def bcast_row(dst, src_b, row_ap):
        k.dma(dst[:], V(src_b, row_ap.to_broadcast([128, D])))

    p0 = ExitStack()
    k.es = p0
    scs = k.sb([128, 8, 2], F32)
    modsb = k.sb([2, 6144], F32)
    brow = k.sb([2, 6144], F32)
    wst = [k.sb([128, 8, 512], F32) for _ in range(2)]
    k.dma(scs[:], scT[:])
    k.act(scs[:], scs[:], AF.Silu)
    k.dma(brow[:], V(b_ada, b_ada.t[0:1, :].to_broadcast([2, 6144])))
    for jg in range(12):
        w_ = wst[jg % 2]
        k.dma(w_[:], V(w_ada, w_ada.t[:, jg * 512:(jg + 1) * 512].rearrange("(c p) j -> p c j", p=128)))
        ps = PS[jg % 2]
        for dc in range(8):
            k.mm(ps[0:2, :], scs[:, dc, :], w_[:, dc, :], start=(dc == 0), stop=(dc == 7))
        k.tt(modsb[:, jg * 512:(jg + 1) * 512], ps[0:2, :], brow[:, jg * 512:(jg + 1) * 512], ALU.add)
    k.dma(mod_d[:], modsb[:])
    bcast_row(r_fg, fg, fg.t[0:1, :])
    for col in range(2):
        bcast_row(r_gt2[col], mod_d, mod_d.t[col:col + 1, 5 * D:6 * D])
    k.barrier()
    p0.close()

    p1 = ExitStack()
    k.es = p1
    r_sh1 = k.sb([128, D], F32); r_sc1 = k.sb([128, D], F32); r_g1 = k.sb([128, D], F32)
    r_sh2 = k.sb([128, D], F32); r_sc2 = k.sb([128, D], F32)
    g1row = k.sb([128, D], F32); g2row = k.sb([128, D], F32)
    bcast_row(g1row, g1, g1.t[0:1, :])
    bcast_row(g2row, g2, g2.t[0:1, :])

    def load_rows(col):
        m = lambda i: mod_d.t[col:col + 1, i * D:(i + 1) * D]
        bcast_row(r_sh1, mod_d, m(0)); bcast_row(r_sc1, mod_d, m(1)); bcast_row(r_g1, mod_d, m(2))
        bcast_row(r_sh2, mod_d, m(3)); bcast_row(r_sc2, mod_d, m(4))
        k.stt(r_sc1[:], r_sc1[:], 1.0, g1row[:], ALU.add, ALU.mult)
        k.stt(r_sc2[:], r_sc2[:], 1.0, g2row[:], ALU.add, ALU.mult)

    load_rows(0)
    xt_b = [k.sb([128, D], F32) for _ in range(2)]
    hb = k.sb([128, D], F32)
    x1b = k.sb([128, D], F32)
    stg = k.sb([128, 4, 8, 128], F32)
    wbf = [k.sb([128, 4, 8, 128], BF16)]
    wo_bf = k.sb([128, 8, D], BF16)
    h1T = k.sb([128, 8, 256], BF16)
    dst_ = [k.sb([128, 8, 256], F32)] * 2
    da_bf = k.sb([128, 8, 256], BF16); dn_bf = k.sb([128, 8, 256], BF16)
    mT = k.sb([128, 8, 256], BF16)
    sA = k.sb([128, 256], F32); sB = k.sb([128, 256], F32); mA = k.sb([128, 256], F32); mB = k.sb([128, 256], F32)

    for hf in range(2):
        stv = stg[:].re("p a c j -> p (a c j)").re("p (c j) -> p c j", c=8)
        k.dma(stv, V(w_o, w_o.t[:, hf * 512:(hf + 1) * 512].rearrange("(c p) j -> p c j", p=128)))
        k.copy(wo_bf[:, :, hf * 512:(hf + 1) * 512], stv, eng="pool")

    def to_T(src, dstT, c0):
        for dc in range(8):
            k.tr(PS[dc // 4][:, (dc % 4) * 128:(dc % 4 + 1) * 128], src[:, dc * 128:(dc + 1) * 128], ident[:])
        for hh in range(2):
            k.copy(dstT[:, hh * 4:(hh + 1) * 4, c0:c0 + 128], PS[hh][:].re("p (c n) -> p c n", c=4),
                   eng=("act" if hh else "dve"))

    blocks = [[2 * i, 2 * i + 1] for i in range(8)] + [[16]]
    wcnt = 0
    for blk in blocks:
        ntok = 128 * len(blk)
        n0 = blk[0] * 128
        if blk[0] == 16:
            load_rows(1)
        for ti, t in enumerate(blk):
            xt = xt_b[ti]
            k.dma(xt[:], x_tok[t * 128:(t + 1) * 128, :])
            rs = norm_stats(xt[:])
            k.stt(hb[:], xt[:], rs, r_sc1[:], ALU.mult, ALU.mult)
            k.tt(hb[:], hb[:], r_sh1[:], ALU.add)
            to_T(hb, h1T, ti * 128)
        for (srcT, dbf, st) in ((daT, da_bf, dst_[0]), (dnT, dn_bf, dst_[1])):
            k.dma(st[:, :, :ntok], V(srcT, srcT.t[:, n0:n0 + ntok].rearrange("(c p) n -> p c n", p=128)))
            k.copy(dbf[:, :, :ntok], st[:, :, :ntok], eng="pool")
        for j in range(8):
            cs = slice(j * 128, (j + 1) * 128)
            srcs = [w_g.t[:, j * 128:(j + 1) * 128], w_g.t[:, 1024 + j * 128:1024 + (j + 1) * 128], w_ba.t[:, cs], w_bb.t[:, cs]]
            owners = [w_g, w_g, w_ba, w_bb]
            for a in range(4):
                k.dma(stg[:, a], V(owners[a], srcs[a].rearrange("(c p) j -> p c j", p=128)))
            wb = wbf[0]
            wcnt += 1
            k.copy(wb[:], stg[:], eng="pool")
            for (gi, pi, act_in, sS, mM, pg, pp) in ((0, 2, da_bf, sA, mA, PS[2], PS[3]), (1, 3, dn_bf, sB, mB, PS[4], PS[5])):
                for dc in range(8):
                    k.mm(pg[:, :ntok], wb[:, gi, dc, :], h1T[:, dc, :ntok], start=(dc == 0), stop=(dc == 7))
                k.act(sS[:, :ntok], pg[:, :ntok], AF.Sigmoid)
                for dc in range(8):
                    k.mm(pp[:, :ntok], wb[:, pi, dc, :], act_in[:, dc, :ntok], start=(dc == 0), stop=(dc == 7))
                k.tt(mM[:, :ntok], pp[:, :ntok], sS[:, :ntok], ALU.mult)
            k.tt(mT[:, j, :ntok], mA[:, :ntok], mB[:, :ntok], ALU.add, eng="pool")
        for ti, t in enumerate(blk):
            xt = xt_b[ti]
            for hf in range(2):
                for j in range(8):
                    k.mm(PS[6 + hf][:], mT[:, j, ti * 128:(ti + 1) * 128], wo_bf[:, j, hf * 512:(hf + 1) * 512],
                         start=(j == 0), stop=(j == 7))
                k.tt(x1b[:, hf * 512:(hf + 1) * 512], PS[6 + hf][:], r_g1[:, hf * 512:(hf + 1) * 512], ALU.mult)
            k.tt(x1b[:], x1b[:], xt[:], ALU.add)
            k.dma(x1_d[t * 128:(t + 1) * 128, :], x1b[:])
            rs = norm_stats(x1b[:])
            k.stt(hb[:], x1b[:], rs, r_sc2[:], ALU.mult, ALU.mult)
            k.tt(hb[:], hb[:], r_sh2[:], ALU.add)
            to_T(hb, h2T, t * 128)
    k.barrier()
    p1.close()

    pq = ExitStack()
    k.es = pq
    stq = k.sb([128, 8, 512], F32)
    wq_bf = k.sb([128, 8, 2048], BF16)
    keys_st = k.sb([128, 16, 128], F32)
    keys_bf = k.sb([128, 16, 128], BF16)
    for i in range(4):
        k.dma(stq[:], V(wq, wq.t[:, i * 512:(i + 1) * 512].rearrange("(c p) j -> p c j", p=128)))
        k.copy(wq_bf[:, :, i * 512:(i + 1) * 512], stq[:], eng="pool")
    k.dma(keys_st[:], keysT[:])
    k.copy(keys_bf[:], keys_st[:], eng="pool")
    qT_sb = k.sb([128, 16, 128], BF16)
    tv = k.sb([128, 16, 16], F32); tiu = k.sb([128, 16, 16], U32); tif = k.sb([128, 16, 16], F32)
    wk = k.sb([128, 128], F32); wk2 = k.sb([128, 256], F32)
    cand = k.sb([128, 8, 256], F32)
    bs = k.sb([128, 8, 16], F32); bp = k.sb([128, 8, 16], U32)
    ai = k.sb([128, 8, 16], U32); bi = k.sb([128, 8, 16], U32)
    af = k.sb([128, 8, 16], F32); bf_ = k.sb([128, 8, 16], F32)
    ex = k.sb([128, 8, 16], F32); se = k.sb([128, 8], F32); rse = k.sb([128, 8], F32)
    gate = k.sb([128, 8, 16], F32)
    oh = k.sb([128, 8, 16, 16], F32)
    i1s = k.sb([128, 8, 16], F32); i2s = k.sb([128, 8, 16], F32)

    def top16(src, tvals, tidx, work):
        k.op("dve", lambda: nc.vector.max(out=tvals[:, 0:8].ap, in_=src.ap), [src], [tvals])
        k.op("dve", lambda: nc.vector.max_index(out=tidx[:, 0:8].ap, in_max=tvals[:, 0:8].ap, in_values=src.ap), [src, tvals], [tidx])
        k.op("dve", lambda: nc.vector.match_replace(out=work.ap, in_to_replace=tvals[:, 0:8].ap, in_values=src.ap, imm_value=-1e30),
             [src, tvals], [work])
        k.op("dve", lambda: nc.vector.max(out=tvals[:, 8:16].ap, in_=work.ap), [work], [tvals])
        k.op("dve", lambda: nc.vector.max_index(out=tidx[:, 8:16].ap, in_max=tvals[:, 8:16].ap, in_values=work.ap), [work, tvals], [tidx])

    for t in range(NT_B):
        ts_ = slice(t * 128, (t + 1) * 128)
        for g4 in range(4):
            pb = PS[g4 % 2]
            for q in range(4):
                hp = g4 * 4 + q
                for dc in range(8):
                    k.mm(pb[:, q * 128:(q + 1) * 128], wq_bf[:, dc, hp * 128:(hp + 1) * 128], h2T[:, dc, ts_],
                         start=(dc == 0), stop=(dc == 7))
            k.copy(qT_sb[:, g4 * 4:(g4 + 1) * 4, :], pb[:].re("p (c n) -> p c n", c=4), eng=("act" if g4 % 2 else "dve"))
        for hp in range(16):
            k.mm(PS[2 + hp // 4][:, (hp % 4) * 128:(hp % 4 + 1) * 128], qT_sb[:, hp, :], keys_bf[:, hp, :])
        for hp in range(16):
            sv = PS[2 + hp // 4][:, (hp % 4) * 128:(hp % 4 + 1) * 128]
            top16(sv, tv[:, hp, :], tiu[:, hp, :], wk[:])
        k.copy(tif[:], tiu[:])
        tv4 = tv[:].re("n (h p) a -> n h p a", p=2)
        k.tt(cand[:].re("n h (a b) -> n h a b", a=16), tv4[:, :, 0, :].us(3).bc([128, 8, 16, 16]),
             tv4[:, :, 1, :].us(2).bc([128, 8, 16, 16]), ALU.add)
        for h in range(8):
            top16(cand[:, h, :], bs[:, h, :], bp[:, h, :], wk2[:])
        k.tt(ex[:], bs[:], bs[:, :, 0:1].bc([128, 8, 16]), ALU.subtract)
        k.act(ex[:], ex[:], AF.Exp)
        k.reduce(se[:], ex[:], ALU.add)
        k.recip(rse[:], se[:])
        k.tt(gate[:], ex[:], rse[:].us(2).bc([128, 8, 16]), ALU.mult)
        k.op("dve", lambda: nc.vector.tensor_single_scalar(out=ai[:].ap, in_=bp[:].ap, scalar=4, op=ALU.logical_shift_right), [bp], [ai])
        k.op("dve", lambda: nc.vector.tensor_single_scalar(out=bi[:].ap, in_=bp[:].ap, scalar=15, op=ALU.bitwise_and), [bp], [bi])
        k.copy(af[:], ai[:])
        k.copy(bf_[:], bi[:])
        tif4 = tif[:].re("n (h p) a -> n h p a", p=2)
        for (sel, pf, dstv) in ((af, 0, i1s), (bf_, 1, i2s)):
            k.tt(oh[:], sel[:].us(3).bc([128, 8, 16, 16]), iota16[:].us(1).us(1).bc([128, 8, 16, 16]), ALU.is_equal)
            k.tt(oh[:], oh[:], tif4[:, :, pf, :].us(2).bc([128, 8, 16, 16]), ALU.mult)
            k.reduce(dstv[:], oh[:], ALU.add)
        for (srcv, dT, pb) in ((i1s, idx1T, PS[6]), (i2s, idx2T, PS[7]), (gate, gateT, PS[6])):
            k.tr(pb[:, 0:128], srcv[:].re("n h a -> n (h a)"), ident[:])
            k.copy(dT[:, ts_], pb[:, 0:128], eng="act")
    k.barrier()
    pq.close()

    pe_ = ExitStack()
    k.es = pe_
    Wbuf = k.sb([128, 256, 128], BF16)
    Am = k.sb([128, 32, 128], BF16); Bm = k.sb([128, 32, 128], BF16)
    ust = [k.sb([128, 8, 128], F32) for _ in range(2)]
    vst = [k.sb([128, D], F32) for _ in range(2)]
    ubf = [k.sb([128, 8, 128], BF16) for _ in range(2)]
    vbf = [k.sb([128, D], BF16) for _ in range(2)]
    gl = [k.sb([128, 256], F32) for _ in range(2)]
    GA = [k.sb([128, 256], BF16) for _ in range(2)]
    x1t = k.sb([128, D], F32); x2b = k.sb([128, D], F32)
    groups = [[2 * i, 2 * i + 1] for i in range(8)] + [[16]]
    for gt in groups:
        G = 128 * len(gt)
        tok0 = gt[0] * 128
        SUB = 32
        for sub in range(G // SUB):
            ng = tok0 + sub * SUB
            io_b = iotaI[:].us(1).bc([128, SUB, 128])
            k.tt(Am[:], io_b, idx1T[:, ng:ng + SUB].us(2).bc([128, SUB, 128]), ALU.is_equal)
            k.tt(Am[:], Am[:], gateT[:, ng:ng + SUB].us(2).bc([128, SUB, 128]), ALU.mult, eng="pool")
            k.tt(Bm[:], io_b, idx2T[:, ng:ng + SUB].us(2).bc([128, SUB, 128]), ALU.is_equal)
            for n in range(SUB):
                slot = n % 4
                pw = PS[6 + (n // 4) % 2]
                k.mm(pw[:, slot * 128:(slot + 1) * 128], Am[:, n, :], Bm[:, n, :])
                if slot == 3:
                    k.copy(Wbuf[:, sub * SUB + n - 3:sub * SUB + n + 1, :], pw[:].re("p (c n) -> p c n", c=4), eng="act")
        for i2 in range(128):
            pb = i2 % 2
            k.dma(ust[pb][:], V(uT, uT.t[i2].rearrange("(c p) i -> p c i", p=128)))
            k.dma(vst[pb][:], vP[i2])
            k.copy(ubf[pb][:], ust[pb][:], eng="pool")
            k.copy(vbf[pb][:], vst[pb][:], eng="pool")
            pa = PS[4 + pb]
            for dc in range(8):
                k.mm(pa[:, :G], ubf[pb][:, dc, :], h2T[:, dc, tok0:tok0 + G], start=(dc == 0), stop=(dc == 7))
            k.act(gl[pb][:, :G], pa[:, :G], AF.Gelu)
            k.tt(GA[pb][:, :G], gl[pb][:, :G], Wbuf[:, :G, i2], ALU.mult)
            for ti in range(len(gt)):
                for hf in range(2):
                    k.mm(PS[ti * 2 + hf][:], GA[pb][:, ti * 128:(ti + 1) * 128], vbf[pb][:, hf * 512:(hf + 1) * 512],
                         start=(i2 == 0), stop=(i2 == 127))
        for ti, t in enumerate(gt):
            col = 1 if t == 16 else 0
            k.dma(x1t[:], x1_d[t * 128:(t + 1) * 128, :])
            for hf in range(2):
                hs = slice(hf * 512, (hf + 1) * 512)
                k.tt(x2b[:, hs], PS[ti * 2 + hf][:], r_gt2[col][:, hs], ALU.mult)
            k.tt(x2b[:], x2b[:], x1t[:], ALU.add)
            k.dma(x2_o[t * 128:(t + 1) * 128, :], x2b[:])
            rs = norm_stats(x2b[:])
            k.stt(x1t[:], x2b[:], rs, r_fg[:], ALU.mult, ALU.mult)
            k.dma(fin_o[t * 128:(t + 1) * 128, :], x1t[:])
    k.barrier()
    k.finish([x2_o, fin_o])
    print("B: insts", k.ninst, "waits", k.nwait)
    pe_.close()
    return nc, es


def prep_B_inputs(inp, layer, x_l, x_c, da_l, da_c, dn_l, dn_c):
    g = lambda k: np.ascontiguousarray(inp[k][layer])
    w_in = inp["w_in"][layer]
    w_g = np.ascontiguousarray(w_in[:, 9248 - 2048:])
    keys = inp["peer_keys"][layer]
    keysT = np.ascontiguousarray(keys.reshape(16, 128, 128).transpose(2, 0, 1))
    u = inp["peer_u"][layer].reshape(128, 128, 1024)
    uT = np.ascontiguousarray(u.transpose(1, 2, 0))
    v = inp["peer_v"][layer].reshape(128, 128, 1024)
    vP = np.ascontiguousarray(v.transpose(1, 0, 2))
    shared = dict(w_ada=g("w_ada"), b_ada=g("b_ada")[None, :], g1=g("norm1_g")[None, :], g2=g("norm2_g")[None, :],
                  fg=np.ascontiguousarray(inp["final_g"])[None, :], w_g=w_g, w_ba=g("w_branch_a"), w_bb=g("w_branch_b"),
                  w_o=g("w_out"), wq=g("peer_wq"), keysT=keysT, uT=uT, vP=vP)
    maps = []
    for core in range(8):
        b, hh = core // 2, core % 2
        ls = slice(hh * 2048, (hh + 1) * 2048); cs = slice(hh * 128, (hh + 1) * 128)
        cat = lambda a_l, a_c: np.concatenate([a_l[b, ls], a_c[b, cs]], axis=0)
        x_tok = np.ascontiguousarray(cat(x_l, x_c))
        daT = np.ascontiguousarray(cat(da_l, da_c).T)
        dnT = np.ascontiguousarray(cat(dn_l, dn_c).T)
        sc = np.stack([inp["c"][b], inp["c_ctx"]], axis=1)
        scT = np.ascontiguousarray(sc.reshape(8, 128, 2).transpose(1, 0, 2))
        m = dict(shared); m.update(x_tok=x_tok, daT=daT, dnT=dnT, scT=scT)
        maps.append(m)
    return maps

def gather_B(results, key):
    x_l = np.zeros((4, 4096, 1024), np.float32); x_c = np.zeros((4, 256, 1024), np.float32)
    for core in range(8):
        b, hh = core // 2, core % 2
        r = results[core][key]
        x_l[b, hh * 2048:(hh + 1) * 2048] = r[:2048]
        x_c[b, hh * 128:(hh + 1) * 128] = r[2048:]
    return x_l, x_c

import math
def rope_tables():
    T = 4352
    n_rows = 64
    row = np.repeat(np.arange(n_rows, dtype=np.float32), 64)
    col = np.concatenate([np.arange(64, dtype=np.float32)] * n_rows)
    n_freq = 16
    inv = (10000.0 ** (-np.arange(n_freq, dtype=np.float32) / n_freq)).astype(np.float32)
    ang = np.concatenate([row[:, None] * inv, col[:, None] * inv], axis=-1).astype(np.float32)
    cos = np.cos(ang).astype(np.float32); sin = np.sin(ang).astype(np.float32)
    cosT = np.ones((128, T), np.float32); sinT = np.zeros((128, T), np.float32)
    for c in range(2):
        for d in range(64):
            r = c * 64 + d
            cosT[r, 256:] = cos[:, d // 2]
            sinT[r, 256:] = sin[:, d // 2] * (-1.0 if d % 2 == 0 else 1.0)
    return cosT, sinT

_ROPE = None
def prep_A_inputs(inp, layer, x_l, x_c):
    global _ROPE
    if _ROPE is None:
        _ROPE = rope_tables()
    cosT, sinT = _ROPE
    g = lambda k: inp[k][layer]
    w_in = g("w_in")
    lam_init = 0.8 - 0.6 * math.exp(-0.3 * layer)
    swap = np.arange(128).reshape(64, 2)[:, ::-1].reshape(-1)
    maps = []
    for core in range(8):
        b, hh = core // 2, core % 2
        heads = [4 * hh + i for i in range(4)]
        xT = np.ascontiguousarray(np.concatenate([x_c[b], x_l[b]], axis=0).T)
        sc = np.stack([inp["c"][b], inp["c_ctx"]], axis=1)
        scT = np.ascontiguousarray(sc.reshape(8, 128, 2).transpose(1, 0, 2))
        hc = lambda base: np.concatenate([w_in[:, base + h * 128: base + (h + 1) * 128] for h in heads], axis=1)
        hcs = lambda base: np.concatenate([w_in[:, base + h * 128: base + (h + 1) * 128][:, swap] for h in heads], axis=1)
        w_att = np.ascontiguousarray(np.concatenate([hc(0), hcs(0), hc(1024), hcs(1024), hc(2048)], axis=1))
        w_dn = np.ascontiguousarray(np.concatenate([hc(3072), hc(4096), hc(5120), hc(6144)], axis=1))
        bcols = [7168 + d * 8 + h for d in range(2) for h in heads] + [7184 + d * 8 + h for d in range(2) for h in heads]
        w_ba16 = np.ascontiguousarray(w_in[:, bcols])
        conv = g("dn_conv")
        cwl = np.zeros((128, 5, 12), np.float32)
        for which in range(3):
            for i, h in enumerate(heads):
                cwl[:, :, which * 4 + i] = conv[:, which * 1024 + h * 128: which * 1024 + (h + 1) * 128].T
        alog = np.ascontiguousarray(g("dn_a_log")[:, heads].reshape(1, 8))
        dtb = np.ascontiguousarray(g("dn_dt_bias")[:, heads].reshape(1, 8))
        m = dict(xT=xT, scT=scT, w_ada01=np.ascontiguousarray(g("w_ada")[:, :2048]),
                 b_adaT=np.ascontiguousarray(g("b_ada")[:2048].reshape(16, 128).T),
                 g1T=np.ascontiguousarray(g("norm1_g").reshape(8, 128).T),
                 w_att=w_att, w_dn=w_dn, w_ba16=w_ba16, cosT=cosT, sinT=sinT,
                 lamv=np.ascontiguousarray(g("da_lambda"))[None], laminit=np.array([[lam_init, 1 - lam_init]], np.float32),
                 subln=np.ascontiguousarray(g("da_subln"))[:, None], cw=cwl, alog=alog, dtb=dtb,
                 dnorm=np.ascontiguousarray(g("dn_norm"))[:, None])
        maps.append(m)
    return maps

def gather_A(results, key):
    a_l = np.zeros((4, 4096, 1024), np.float32); a_c = np.zeros((4, 256, 1024), np.float32)
    for core in range(8):
        b, hh = core // 2, core % 2
        r = results[core][key]
        a_c[b, :, hh * 512:(hh + 1) * 512] = r[:, :256].T
        a_l[b, :, hh * 512:(hh + 1) * 512] = r[:, 256:].T
    return a_l, a_c


_PROGS = {}


def _prog(name):
    if name not in _PROGS:
        _PROGS[name] = build_A() if name == "A" else build_B()
    return _PROGS[name][0]


def kernel(**inputs):
    inp = {k_: np.asarray(v_) for k_, v_ in inputs.items()}
    x_l = np.asarray(inp["x"], np.float32)
    x_c = np.asarray(inp["ctx"], np.float32)
    cores = list(range(8))
    fin = None
    for layer in range(2):
        ncA = _prog("A")
        mapsA = prep_A_inputs(inp, layer, x_l, x_c)
        resA = run_bass_kernel_spmd(ncA, mapsA, core_ids=cores)
        da_l, da_c = gather_A(resA.results, "daT_o")
        dn_l, dn_c = gather_A(resA.results, "dnT_o")
        del resA, mapsA
        ncB = _prog("B")
        mapsB = prep_B_inputs(inp, layer, x_l, x_c, da_l, da_c, dn_l, dn_c)
        resB = run_bass_kernel_spmd(ncB, mapsB, core_ids=cores)
        x_l, x_c = gather_B(resB.results, "x2_o")
        if layer == 1:
            fin, _ = gather_B(resB.results, "fin_o")
        del resB, mapsB
    return fin.astype(np.float32)
```
